# Optimizing a Trainium2 kernel written in Bass

```python
import jax, jax.numpy as jnp
from jax import lax
import numpy as np

D_MODEL = 2048
BATCH = 4
SEQ = 2048
DEPTH = 1

CTX_LEN = 256
GRID_W = 64
EPS = 1e-6

M_HEADS = 8
M_DQK = 128
M_DV = 256
M_CHUNK = 64

A_HEADS = 16
A_KV_HEADS = 4
A_GROUP = A_HEADS // A_KV_HEADS
A_DH = 128
WINDOW = 128
A_BLOCK = 128
ROPE_HALF = A_DH // 4
ROPE_BASE = 10000.0

N_GROUPS = 8
EXPERTS_PER_GROUP = 8
N_EXPERTS = N_GROUPS * EXPERTS_PER_GROUP
TOP_K = 2
D_EXPERT = 1024
MOE_BLOCK = 128

M_QK_W = M_HEADS * M_DQK
M_V_W = M_HEADS * M_DV
A_Q_W = A_HEADS * A_DH
A_KV_W = A_KV_HEADS * A_DH
IN_SIZES = (M_QK_W, M_QK_W, M_V_W, M_V_W, 4 * M_HEADS, A_Q_W, A_KV_W, A_KV_W, 2 * D_MODEL)
IN_WIDTH = sum(IN_SIZES)
IN_SPLITS = [sum(IN_SIZES[:i]) for i in range(1, len(IN_SIZES))]

kernel_name = "hybrid_mlstm_swa_hmoe_prefix_block"

F32 = jnp.float32


def rms_norm(x, g):
    xf = x.astype(F32)
    y = xf * lax.rsqrt(jnp.mean(xf * xf, axis=-1, keepdims=True) + EPS)
    return (y * g.astype(F32)).astype(x.dtype)


def adaln(cvec, w_mod, b_mod):
    return jnp.split(jax.nn.silu(cvec) @ w_mod + b_mod, 6, axis=-1)


def modulate(h, shift, scale):
    return h * (1 + scale) + shift


def to_heads(t, n_heads, d):
    b, n, _ = t.shape
    return t.reshape(b, n, n_heads, d).transpose(0, 2, 1, 3)


def from_heads(t):
    b, h, n, d = t.shape
    return t.transpose(0, 2, 1, 3).reshape(b, n, h * d)


def flip_t(t):
    return jnp.flip(t, axis=2)


def axial_rope_tables(n_tokens):
    rows = n_tokens // GRID_W
    row = jnp.repeat(jnp.arange(rows, dtype=F32), GRID_W)
    col = jnp.tile(jnp.arange(GRID_W, dtype=F32), rows)
    inv_freq = ROPE_BASE ** (-jnp.arange(ROPE_HALF, dtype=F32) / ROPE_HALF)
    ang = jnp.stack([row[:, None] * inv_freq, col[:, None] * inv_freq], axis=1)
    return jnp.cos(ang), jnp.sin(ang)


def apply_axial_rope(t, cos, sin):
    b, h, n, d = t.shape
    tt = t.astype(F32).reshape(b, h, n, 2, 2, ROPE_HALF)
    t1, t2 = tt[..., 0, :], tt[..., 1, :]
    out = jnp.stack([t1 * cos - t2 * sin, t2 * cos + t1 * sin], axis=-2)
    return out.reshape(b, h, n, d).astype(t.dtype)


def mlstm_zero_state(b):
    return (jnp.zeros((b, M_HEADS, M_DQK, M_DV), F32),
            jnp.zeros((b, M_HEADS, M_DQK), F32),
            jnp.zeros((b, M_HEADS), F32))


def _mlstm_chunk_step(carry, inp):
    C, n, m = carry
    q, k, v, ig, lf = inp
    L = q.shape[2]
    b = jnp.cumsum(lf, axis=-1)
    d = b[..., :, None] - b[..., None, :] + ig[..., None, :]
    d = jnp.where(jnp.tril(jnp.ones((L, L), bool)), d, -jnp.inf)
    inter = b + m[..., None]
    m_t = jnp.maximum(inter, jnp.max(d, axis=-1))
    w_inter = jnp.exp(inter - m_t)
    s = jnp.einsum('bhtk,bhsk->bhts', q, k) * jnp.exp(d - m_t[..., None])
    num = w_inter[..., None] * jnp.einsum('bhtk,bhkv->bhtv', q, C) + jnp.einsum('bhts,bhsv->bhtv', s, v)
    qn = w_inter * jnp.einsum('bhtk,bhk->bht', q, n) + jnp.sum(s, axis=-1)
    h = num / jnp.maximum(jnp.abs(qn), jnp.exp(-m_t))[..., None]
    b_end = b[..., -1]
    dec = b_end[..., None] - b + ig
    m_new = jnp.maximum(b_end + m, jnp.max(dec, axis=-1))
    w_s = jnp.exp(dec - m_new[..., None])
    w_c = jnp.exp(b_end + m - m_new)
    C_new = w_c[..., None, None] * C + jnp.einsum('bhs,bhsk,bhsv->bhkv', w_s, k, v)
    n_new = w_c[..., None] * n + jnp.einsum('bhs,bhsk->bhk', w_s, k)
    return (C_new, n_new, m_new), h


def mlstm_scan(q, k, v, ig, lf, state):
    b, h, t, _ = q.shape
    nc = t // M_CHUNK

    def chunks(a):
        return jnp.moveaxis(a.reshape(b, h, nc, M_CHUNK, *a.shape[3:]), 2, 0)

    state, hs = lax.scan(_mlstm_chunk_step, state, (chunks(q), chunks(k), chunks(v), chunks(ig), chunks(lf)))
    return jnp.moveaxis(hs, 0, 2).reshape(b, h, t, M_DV), state


def mlstm_inputs(q, k, v, g, gate_b):
    b, t, _ = q.shape
    q = to_heads(q, M_HEADS, M_DQK).astype(F32)
    k = to_heads(k, M_HEADS, M_DQK).astype(F32) * (M_DQK ** -0.5)
    v = to_heads(v, M_HEADS, M_DV).astype(F32)
    g = g.reshape(b, t, 4, M_HEADS).astype(F32) + gate_b.astype(F32)
    g = jnp.transpose(g, (2, 0, 3, 1))
    return q, k, v, g


def bidir_mlstm(lat, cx, need_ctx_out):
    q, k, v, g = lat
    qc, kc, vc, gc = cx
    zero = mlstm_zero_state(q.shape[0])
    lsig = jax.nn.log_sigmoid
    hf_c, st_f = mlstm_scan(qc, kc, vc, gc[0], lsig(gc[1]), zero)
    hb_c, st_b = mlstm_scan(flip_t(qc), flip_t(kc), flip_t(vc), flip_t(gc[2]), flip_t(lsig(gc[3])), zero)
    hf, _ = mlstm_scan(q, k, v, g[0], lsig(g[1]), st_f)
    hb, _ = mlstm_scan(flip_t(q), flip_t(k), flip_t(v), flip_t(g[2]), flip_t(lsig(g[3])), st_b)
    h_lat = hf + flip_t(hb)
    h_ctx = hf_c + flip_t(hb_c) if need_ctx_out else None
    return h_lat, h_ctx


def mlstm_output(hh, o, norm_g, w_br):
    hh = hh * lax.rsqrt(jnp.mean(hh * hh, axis=-1, keepdims=True) + EPS)
    hh = from_heads(hh).astype(o.dtype) * norm_g * jax.nn.sigmoid(o)
    return hh @ w_br


def windowed_gqa_with_context(q, k, v, kc, vc, sink):
    b, _, s, _ = q.shape
    nb = s // A_BLOCK
    n_ctx = kc.shape[2]
    scale = A_DH ** -0.5
    qb = q.reshape(b, A_KV_HEADS, A_GROUP, nb, A_BLOCK, A_DH)

    def band(t):
        tb = jnp.pad(t, ((0, 0), (0, 0), (A_BLOCK, A_BLOCK), (0, 0))).reshape(b, A_KV_HEADS, nb + 2, A_BLOCK, A_DH)
        return jnp.concatenate([tb[:, :, :-2], tb[:, :, 1:-1], tb[:, :, 2:]], axis=3)

    kb, vb = band(k), band(v)
    s_loc = jnp.einsum('bhgnqd,bhnkd->bhgnqk', qb, kb).astype(F32) * scale
    s_ctx = jnp.einsum('bhgnqd,bhcd->bhgnqc', qb, kc).astype(F32) * scale
    blk = jnp.arange(nb)[:, None, None]
    qpos = blk * A_BLOCK + jnp.arange(A_BLOCK)[None, :, None]
    kpos = (blk - 1) * A_BLOCK + jnp.arange(3 * A_BLOCK)[None, None, :]
    valid = (jnp.abs(qpos - kpos) <= WINDOW) & (kpos >= 0) & (kpos < s)
    s_loc = jnp.where(valid, s_loc, -jnp.inf)
    s_sink = jnp.broadcast_to(sink.astype(F32).reshape(1, A_KV_HEADS, A_GROUP, 1, 1, 1), s_loc.shape[:-1] + (1,))
    p = jax.nn.softmax(jnp.concatenate([s_loc, s_ctx, s_sink], axis=-1), axis=-1)
    p_loc = p[..., :3 * A_BLOCK].astype(v.dtype)
    p_ctx = p[..., 3 * A_BLOCK:3 * A_BLOCK + n_ctx].astype(v.dtype)
    o = jnp.einsum('bhgnqk,bhnkd->bhgnqd', p_loc, vb) + jnp.einsum('bhgnqc,bhcd->bhgnqd', p_ctx, vc)
    return o.reshape(b, A_HEADS, s, A_DH)


def context_attention(qc, kc, vc, sink):
    b, _, t, _ = qc.shape
    qg = qc.reshape(b, A_KV_HEADS, A_GROUP, t, A_DH)
    sc = jnp.einsum('bhgqd,bhcd->bhgqc', qg, kc).astype(F32) * (A_DH ** -0.5)
    s_sink = jnp.broadcast_to(sink.astype(F32).reshape(1, A_KV_HEADS, A_GROUP, 1, 1), sc.shape[:-1] + (1,))
    p = jax.nn.softmax(jnp.concatenate([sc, s_sink], axis=-1), axis=-1)[..., :-1]
    o = jnp.einsum('bhgqc,bhcd->bhgqd', p.astype(vc.dtype), vc)
    return o.reshape(b, A_HEADS, t, A_DH)


def merge_branches(br_m, br_a, gbr, w_out):
    g_m, g_a = jnp.split(jax.nn.sigmoid(gbr), 2, axis=-1)
    return (g_m * br_m + g_a * br_a) @ w_out


def token_mixers(h, hc, cos, sin, w_in, gate_b, m_norm_g, sink, w_br_m, w_br_a, w_out, update_ctx):
    qm, km, vm, om, gm, qa, ka, va, gbr = jnp.split(h @ w_in, IN_SPLITS, axis=-1)
    qmc, kmc, vmc, omc, gmc, qac, kac, vac, gbrc = jnp.split(hc @ w_in, IN_SPLITS, axis=-1)
    hm, hmc = bidir_mlstm(mlstm_inputs(qm, km, vm, gm, gate_b), mlstm_inputs(qmc, kmc, vmc, gmc, gate_b), update_ctx)
    br_m = mlstm_output(hm, om, m_norm_g, w_br_m)
    q = apply_axial_rope(to_heads(qa, A_HEADS, A_DH), cos, sin)
    k = apply_axial_rope(to_heads(ka, A_KV_HEADS, A_DH), cos, sin)
    v = to_heads(va, A_KV_HEADS, A_DH)
    kc = to_heads(kac, A_KV_HEADS, A_DH)
    vc = to_heads(vac, A_KV_HEADS, A_DH)
    br_a = from_heads(windowed_gqa_with_context(q, k, v, kc, vc, sink)) @ w_br_a
    y = merge_branches(br_m, br_a, gbr, w_out)
    yc = None
    if update_ctx:
        br_mc = mlstm_output(hmc, omc, m_norm_g, w_br_m)
        br_ac = from_heads(context_attention(to_heads(qac, A_HEADS, A_DH), kc, vc, sink)) @ w_br_a
        yc = merge_branches(br_mc, br_ac, gbrc, w_out)
    return y, yc


def hierarchical_moe(h, w_rg, b_rg, w_re, b_re, w_g, w_u, w_d):
    t_tok, d = h.shape
    grp_logits = (h @ w_rg + b_rg).astype(F32)
    p_grp = jax.nn.softmax(grp_logits, axis=-1)
    g_sel = jnp.argmax(grp_logits, axis=-1)
    exp_logits = (h @ w_re + b_re).astype(F32).reshape(t_tok, N_GROUPS, EXPERTS_PER_GROUP)
    in_grp = jnp.take_along_axis(exp_logits, g_sel[:, None, None], axis=1)[:, 0]
    top_val, top_idx = lax.top_k(in_grp, TOP_K)
    w_top = jax.nn.softmax(top_val, axis=-1) * jnp.take_along_axis(p_grp, g_sel[:, None], axis=1)
    eid = (g_sel[:, None] * EXPERTS_PER_GROUP + top_idx).reshape(-1)
    n_assign = t_tok * TOP_K
    order = jnp.argsort(eid)
    sorted_eid = eid[order]
    tok = order // TOP_K
    w_sorted = w_top.reshape(-1)[order]
    counts = jnp.bincount(eid, length=N_EXPERTS)
    padded = ((counts + MOE_BLOCK - 1) // MOE_BLOCK) * MOE_BLOCK
    start = jnp.cumsum(counts) - counts
    pend = jnp.cumsum(padded)
    pstart = pend - padded
    dest = pstart[sorted_eid] + jnp.arange(n_assign) - start[sorted_eid]
    n_blocks = -(-(n_assign + N_EXPERTS * (MOE_BLOCK - 1)) // MOE_BLOCK)
    xbuf = jnp.zeros((n_blocks * MOE_BLOCK, d), h.dtype).at[dest].set(h[tok])
    blk_expert = jnp.minimum(jnp.searchsorted(pend, jnp.arange(n_blocks) * MOE_BLOCK, side='right'), N_EXPERTS - 1)

    def run_block(args):
        xb, e = args
        return (jax.nn.silu(xb @ w_g[e]) * (xb @ w_u[e])) @ w_d[e]

    ybuf = lax.map(run_block, (xbuf.reshape(n_blocks, MOE_BLOCK, d), blk_expert)).reshape(-1, d)
    contrib = ybuf[dest] * w_sorted[:, None].astype(h.dtype)
    return jax.ops.segment_sum(contrib, tok, num_segments=t_tok)


def setup_inputs(seed: int = 0) -> dict:
    key = jax.random.key(seed)
    ks = jax.random.split(key, 24)
    nrm = jax.random.normal
    D = D_MODEL
    return {
        "x": nrm(ks[0], (BATCH, SEQ, D), F32),
        "c": nrm(ks[1], (BATCH, D), F32),
        "ctx": nrm(ks[2], (BATCH, CTX_LEN, D), F32),
        "c_ctx": nrm(ks[3], (D,), F32),
        "w_mod": nrm(ks[4], (DEPTH, D, 6 * D), F32) * D ** -0.5,
        "b_mod": 0.02 * nrm(ks[5], (DEPTH, 6 * D), F32),
        "norm1_g": 1.0 + 0.05 * nrm(ks[6], (DEPTH, D), F32),
        "w_in": nrm(ks[7], (DEPTH, D, IN_WIDTH), F32) * D ** -0.5,
        "mlstm_gate_b": jnp.array([0.0, 3.0, 0.0, 3.0], F32)[None, :, None] + 0.3 * nrm(ks[8], (DEPTH, 4, M_HEADS), F32),
        "mlstm_norm_g": 1.0 + 0.05 * nrm(ks[9], (DEPTH, M_V_W), F32),
        "attn_sink": 0.5 * nrm(ks[10], (DEPTH, A_HEADS), F32),
        "w_br_m": nrm(ks[11], (DEPTH, M_V_W, D), F32) * M_V_W ** -0.5,
        "w_br_a": nrm(ks[12], (DEPTH, A_Q_W, D), F32) * A_Q_W ** -0.5,
        "w_out": nrm(ks[13], (DEPTH, D, D), F32) * D ** -0.5,
        "norm2_g": 1.0 + 0.05 * nrm(ks[14], (DEPTH, D), F32),
        "w_router_grp": nrm(ks[15], (DEPTH, D, N_GROUPS), F32) * D ** -0.5,
        "b_router_grp": 0.01 * nrm(ks[16], (DEPTH, N_GROUPS), F32),
        "w_router_exp": nrm(ks[17], (DEPTH, D, N_EXPERTS), F32) * D ** -0.5,
        "b_router_exp": 0.01 * nrm(ks[18], (DEPTH, N_EXPERTS), F32),
        "w_exp_gate": nrm(ks[19], (DEPTH, N_EXPERTS, D, D_EXPERT), F32) * D ** -0.5,
        "w_exp_up": nrm(ks[20], (DEPTH, N_EXPERTS, D, D_EXPERT), F32) * D ** -0.5,
        "w_exp_down": nrm(ks[21], (DEPTH, N_EXPERTS, D_EXPERT, D), F32) * D_EXPERT ** -0.5,
        "final_norm_g": 1.0 + 0.05 * nrm(ks[22], (D,), F32),
    }


def reference(x, c, ctx, c_ctx, w_mod, b_mod, norm1_g, w_in, mlstm_gate_b, mlstm_norm_g, attn_sink,
              w_br_m, w_br_a, w_out, norm2_g, w_router_grp, b_router_grp, w_router_exp, b_router_exp,
              w_exp_gate, w_exp_up, w_exp_down, final_norm_g):
    b, s, d = x.shape
    n_ctx = ctx.shape[1]
    cos, sin = axial_rope_tables(s)
    xc = ctx
    for l in range(DEPTH):
        update_ctx = l < DEPTH - 1
        sh1, sc1, g1, sh2, sc2, g2 = [m[:, None, :] for m in adaln(c, w_mod[l], b_mod[l])]
        sh1c, sc1c, g1c, sh2c, sc2c, g2c = adaln(c_ctx, w_mod[l], b_mod[l])
        h = modulate(rms_norm(x, norm1_g[l]), sh1, sc1)
        hc = modulate(rms_norm(xc, norm1_g[l]), sh1c, sc1c)
        y, yc = token_mixers(h, hc, cos, sin, w_in[l], mlstm_gate_b[l], mlstm_norm_g[l], attn_sink[l],
                             w_br_m[l], w_br_a[l], w_out[l], update_ctx)
        x = x + g1 * y
        h = modulate(rms_norm(x, norm2_g[l]), sh2, sc2)
        moe_args = (w_router_grp[l], b_router_grp[l], w_router_exp[l], b_router_exp[l],
                    w_exp_gate[l], w_exp_up[l], w_exp_down[l])
        if update_ctx:
            xc = xc + g1c * yc
            hc = modulate(rms_norm(xc, norm2_g[l]), sh2c, sc2c)
            out = hierarchical_moe(jnp.concatenate([hc.reshape(-1, d), h.reshape(-1, d)], axis=0), *moe_args)
            xc = xc + g2c * out[:b * n_ctx].reshape(b, n_ctx, d)
            x = x + g2 * out[b * n_ctx:].reshape(b, s, d)
        else:
            x = x + g2 * hierarchical_moe(h.reshape(-1, d), *moe_args).reshape(b, s, d)
    return rms_norm(x, final_norm_g)
```

```python
import math
from contextlib import ExitStack
import numpy as np
import concourse.bass as bass
import concourse.mybir as mybir
from concourse.bass_utils import run_bass_kernel_spmd

F32 = mybir.dt.float32
BF16 = mybir.dt.bfloat16
AF = mybir.ActivationFunctionType
ALU = mybir.AluOpType
AX = mybir.AxisListType

D = 2048
NDC = 16
SEQ = 2048
OWN = 1024
CTX = 256
NT_ALL = 18
EPS = 1e-6
NEG = -30000.0

ENGS = ("pe", "act", "dve", "pool", "sp")


class T:
    __slots__ = ("name", "w", "r", "rd", "ld_sem", "ld_cnt", "st_sem", "st_cnt", "excl")

    def __init__(self, name, excl=False):
        self.name = name
        self.excl = excl
        self.w = None
        self.r = {}
        self.rd = []
        self.ld_sem = None
        self.ld_cnt = 0
        self.st_sem = None
        self.st_cnt = 0


class Op:
    __slots__ = ("eng", "fn", "deps", "idx", "sig", "sigval", "dma", "sem", "semval")

    def __init__(self, eng, fn):
        self.eng = eng
        self.fn = fn
        self.deps = []
        self.idx = -1
        self.sig = False
        self.sigval = 0
        self.dma = False
        self.sem = None
        self.semval = 0


def _needs_wait(op, d):
    if d.eng != op.eng:
        return True
    if op.dma:
        return True
    if op.eng == "pe":
        return False
    return (op.idx - d.idx) <= 2


class Prog:
    def __init__(self, nc):
        self.nc = nc
        self.ops = {e: [] for e in ENGS}
        self.sem_cms = []
        self.eng_sem = {}
        self.live_dmas = []

    def new_sem(self, name):
        cm = self.nc.semaphore(name)
        s = cm.__enter__()
        self.sem_cms.append(cm)
        return s

    def _add(self, op, reads, writes, extra):
        deps = []
        for t in reads:
            if t.w is not None:
                deps.append(t.w)
            if t.excl:
                deps.extend(o_ for e_, o_ in t.r.items() if e_ != op.eng)
        for t in writes:
            if t.w is not None:
                deps.append(t.w)
            deps.extend(t.r.values())
            deps.extend(t.rd)
        if extra:
            deps.extend(extra)
        op.deps = [d for d in deps if d is not op]
        for t in reads:
            if t in writes:
                continue
            if op.dma:
                t.rd.append(op)
            else:
                t.r[op.eng] = op
        for t in writes:
            t.w = op
            t.r = {}
            t.rd = []
        op.idx = len(self.ops[op.eng])
        self.ops[op.eng].append(op)
        return op

    def op(self, eng, fn, reads=(), writes=(), deps=None):
        return self._add(Op(eng, fn), list(reads), list(writes), deps)

    def dma(self, eng, out_ap, in_ap, reads=(), writes=(), owner=None, store=False, deps=None):
        op = Op(eng, lambda e: e.dma_start(out=out_ap, in_=in_ap))
        op.dma = True
        if store:
            if owner.st_sem is None:
                owner.st_sem = self.new_sem("st_" + owner.name)
            owner.st_cnt += 16
            op.sem, op.semval = owner.st_sem, owner.st_cnt
        else:
            if owner.ld_sem is None:
                owner.ld_sem = self.new_sem("ld_" + owner.name)
            owner.ld_cnt += 16
            op.sem, op.semval = owner.ld_sem, owner.ld_cnt
        self.live_dmas.append(op)
        return self._add(op, list(reads), list(writes), list(deps or []))

    def barrier(self):
        lasts = [self.ops[e][-1] for e in ENGS if self.ops[e] and not self.ops[e][-1].dma]
        lasts = []
        for e in ENGS:
            for o in reversed(self.ops[e]):
                if not o.dma and o.fn is not None:
                    lasts.append(o)
                    break
        dm = {}
        for o in self.live_dmas:
            k = id(o.sem)
            if k not in dm or dm[k].semval < o.semval:
                dm[k] = o
        self.live_dmas = []
        for e in ENGS:
            m = Op(e, None)
            m.deps = [o for o in lasts if o.eng != e] + list(dm.values())
            m.idx = len(self.ops[e])
            self.ops[e].append(m)

    def finalize(self, final_waits=()):
        for e in ENGS:
            self.eng_sem[e] = self.new_sem("eng_" + e)
        for e in ENGS:
            for op in self.ops[e]:
                for d in op.deps:
                    if not d.dma and _needs_wait(op, d):
                        d.sig = True
        for e in ENGS:
            c = 0
            for op in self.ops[e]:
                if op.sig:
                    c += 1
                    op.sigval = c
        prog = self

        def emit(engname, engine):
            waited = {}
            for op in prog.ops[engname]:
                dmax = {}
                for d in op.deps:
                    if d.dma:
                        key = ("d", id(d.sem))
                        if key not in dmax or dmax[key].semval < d.semval:
                            dmax[key] = d
                for key, d in dmax.items():
                    if waited.get(key, 0) < d.semval:
                        engine.wait_ge(d.sem, d.semval)
                        waited[key] = d.semval
                for d in op.deps:
                    if d.dma:
                        continue
                    else:
                        if not _needs_wait(op, d):
                            continue
                        key = ("e", d.eng)
                        if waited.get(key, 0) < d.sigval:
                            engine.wait_ge(prog.eng_sem[d.eng], d.sigval)
                            waited[key] = d.sigval
                if op.fn is None:
                    continue
                ins = op.fn(engine)
                if op.dma:
                    ins.then_inc(op.sem, 16)
                elif op.sig:
                    ins.then_inc(prog.eng_sem[engname], 1)
            if engname == "sp":
                dmax = {}
                for d in final_waits:
                    key = ("d", id(d.sem))
                    if key not in dmax or dmax[key].semval < d.semval:
                        dmax[key] = d
                for key, d in dmax.items():
                    engine.wait_ge(d.sem, d.semval)

        with self.nc.Block() as block:
            @block.tensor
            def _(eng):
                emit("pe", eng)

            @block.scalar
            def _(eng):
                emit("act", eng)

            @block.vector
            def _(eng):
                emit("dve", eng)

            @block.gpsimd
            def _(eng):
                emit("pool", eng)

            @block.sync
            def _(eng):
                emit("sp", eng)

    def close(self):
        for cm in reversed(self.sem_cms):
            cm.__exit__(None, None, None)


M_QK_W, M_V_W, A_Q_W, A_KV_W = 1024, 2048, 2048, 512
OFF_QM = 0
OFF_KM = 1024
OFF_VM = 2048
OFF_OM = 4096
OFF_GM = 6144
OFF_QA = 6176
OFF_KA = 8224
OFF_VA = 8736
OFF_GBR = 9248


def pmaj(w):
    n = w.shape[1]
    return np.ascontiguousarray(w.reshape(16, 128, n).transpose(1, 0, 2).reshape(128, 16 * n))


def rope_perm(n_heads):
    idx = []
    for h in range(n_heads):
        base = h * 128
        for ax in range(2):
            b2 = base + ax * 64
            idx.extend(range(b2 + 32, b2 + 64))
            idx.extend(range(b2, b2 + 32))
    return np.array(idx)


def build(stage=99, dbg=None, nmh=8, nag=4, nexp=64, fast=False, sub=0):
    nc = bass.Bass("TRN2", target_bir_lowering=False)
    es = ExitStack()

    def din(name, shape, dt=F32):
        return nc.dram_tensor(name, list(shape), dt, kind="ExternalInput").ap()

    xl = din("xl", [SEQ, D])
    ctxl = din("ctxl", [CTX, D])
    cc = din("cc", [128, 32])
    wmod = din("wmod", [24, 128, 16 * 512])
    vecs = din("vecs", [128, 224])
    rows = din("rows", [128, 128])
    consts = din("consts", [128, 1280])
    out = nc.dram_tensor("out", [OWN, D], F32, kind="ExternalOutput").ap()
    zkind = "ExternalOutput" if dbg else "Internal"
    zsrc = nc.dram_tensor("zsrc", [32, 128, OWN], BF16, kind=zkind).ap()
    x1s = nc.dram_tensor("x1s", [OWN, D], F32, kind=zkind).ap()

    P = Prog(nc)

    big = es.enter_context(nc.sbuf_tensor("big", [128, 53000], F32))
    psb = [es.enter_context(nc.psum_tensor("ps%d" % i, [128, 512], F32)) for i in range(8)]
    Tps = [T("ps%d" % i, excl=True) for i in range(8)]

    def fv(off, n):
        return big[:, off:off + n]

    def bv(off, n):
        return big[:, off // 2:(off + n) // 2].bitcast(BF16)

    cst = fv(0, 1280)
    vec_sb = fv(1280, 224)
    rows_sb = fv(1504, 128)
    modT = fv(1632, 192)
    AB = fv(1824, 128).rearrange("p (a b) -> p a b", b=16)
    small = fv(1952, 64)
    HT0 = 2048
    hT = bv(HT0 * 2, NDC * (SEQ + CTX)).rearrange("p (a b) -> p a b", b=SEQ + CTX)
    PH = HT0 + 18432

    def pf(off, n):
        assert off + n <= 32520
        return fv(PH + off, n)

    def pb(off_f, n_b):
        assert off_f + (n_b + 1) // 2 <= 32520
        return bv((PH + off_f) * 2, n_b)

    ident = cst[:, 0:128]
    triF = cst[:, 128:256]
    triB = cst[:, 256:384]
    ones = cst[:, 384:512]
    mask3 = cst[:, 512:896]
    mask3f = cst[:, 896:1280]

    T_hT = [T("hT%d" % j) for j in range(NT_ALL)]
    T_modT, T_vec, T_rows, T_cst, T_AB, T_small = T("modT"), T("vec"), T("rows"), T("cst"), T("AB"), T("small")

    P.dma("sp", vec_sb, vecs, writes=[T_vec], owner=T_vec)
    P.dma("sp", rows_sb, rows, writes=[T_rows], owner=T_rows)
    P.dma("sp", cst, consts, writes=[T_cst], owner=T_cst)
    P.op("dve", lambda e: e.memset(small[:, 0:1], EPS), writes=[T_small])
    P.op("dve", lambda e: e.memset(small[:, 1:2], -0.5 * math.log(128.0)), writes=[T_small])

    final_waits = []

    def finish(waits):
        with nc.allow_low_precision("bf16 matmuls"):
            P.finalize(waits)
        P.close()
        es.close()
        return nc

    if fast:
        P.op("dve", lambda e: e.memset(fv(HT0, 18432), 0.25), writes=T_hT)
        P.op("dve", lambda e: e.memset(fv(1824, 128), 0.5), writes=[T_AB])
        P.barrier()
    else:
        cc_sb = pf(0, 32)
        sc_sb = pf(32, 32)
        T_cc, T_sc = T("cc"), T("sc")
        wmb = [pf(64 + i * 8192, 8192).rearrange("p (a b) -> p a b", b=512) for i in range(2)]
        T_wmb = [T("wmb0"), T("wmb1")]
        P.dma("sp", cc_sb, cc, writes=[T_cc], owner=T_cc)
        P.op("act", lambda e: e.activation(out=sc_sb, in_=cc_sb, func=AF.Silu), reads=[T_cc], writes=[T_sc])
        for blk in range(24):
            wm = wmb[blk % 2]
            tw = T_wmb[blk % 2]
            P.dma("sp", wm, wmod[blk].rearrange("p (a b) -> p a b", b=512), writes=[tw], owner=tw)
            for jj in range(4):
                jc = blk * 4 + jj
                for dc in range(NDC):
                    P.op("pe", (lambda e, wm=wm, jj=jj, jc=jc, dc=dc: e.matmul(
                        psb[0][:, jc * 2:(jc + 1) * 2], lhsT=wm[:, dc, jj * 128:(jj + 1) * 128],
                        rhs=sc_sb[:, dc * 2:(dc + 1) * 2], start=(dc == 0), stop=(dc == NDC - 1))),
                        reads=[tw, T_sc], writes=[Tps[0]])
        P.op("dve", lambda e: e.tensor_tensor(out=modT, in0=psb[0][:, 0:192], in1=vec_sb[:, 0:192], op=ALU.add),
             reads=[Tps[0], T_vec], writes=[T_modT])
        mv = modT.rearrange("p (j w) -> p j w", w=2)

        def modv(k, w):
            return mv[:, k * 16:(k + 1) * 16, w]

        n1 = vec_sb[:, 192:208]
        n2 = vec_sb[:, 208:224]
        P.op("dve", lambda e: e.scalar_tensor_tensor(out=AB[:, 0, :], in0=modv(1, 0), scalar=1.0, in1=n1, op0=ALU.add, op1=ALU.mult),
             reads=[T_modT, T_vec], writes=[T_AB])
        P.op("dve", lambda e: e.tensor_copy(out=AB[:, 1, :], in_=modv(0, 0)), reads=[T_modT], writes=[T_AB])
        P.op("dve", lambda e: e.scalar_tensor_tensor(out=AB[:, 2, :], in0=modv(1, 1), scalar=1.0, in1=n1, op0=ALU.add, op1=ALU.mult),
             reads=[T_modT, T_vec], writes=[T_AB])
        P.op("dve", lambda e: e.tensor_copy(out=AB[:, 3, :], in_=modv(0, 1)), reads=[T_modT], writes=[T_AB])
        P.op("dve", lambda e: e.scalar_tensor_tensor(out=AB[:, 4, :], in0=modv(4, 0), scalar=1.0, in1=n2, op0=ALU.add, op1=ALU.mult),
             reads=[T_modT, T_vec], writes=[T_AB])
        P.op("dve", lambda e: e.tensor_copy(out=AB[:, 5, :], in_=modv(3, 0)), reads=[T_modT], writes=[T_AB])
        P.op("dve", lambda e: e.tensor_copy(out=AB[:, 6, :], in_=modv(2, 0)), reads=[T_modT], writes=[T_AB])
        P.op("dve", lambda e: e.tensor_copy(out=AB[:, 7, :], in_=modv(5, 0)), reads=[T_modT], writes=[T_AB])
        P.barrier()

        def norm_to_T(src_rows_ap, xt, T_xt, xn, T_xn, junk, T_junk, stat, T_stat, sidx, A_ap, B_ap, dst_fn, T_dst, ps_ids, load=True):
            if load:
                P.dma("sp", xt, src_rows_ap, writes=[T_xt], owner=T_xt)
            P.op("act", lambda e: e.activation(out=junk, in_=xt, func=AF.Square, accum_out=stat[:, sidx:sidx + 1]),
                 reads=[T_xt], writes=[T_junk, T_stat])
            P.op("act", lambda e: e.activation(out=stat[:, sidx + 1:sidx + 2], in_=stat[:, sidx:sidx + 1], func=AF.Sqrt,
                                               bias=small[:, 0:1], scale=1.0 / D),
                 reads=[T_stat, T_small], writes=[T_stat])
            P.op("dve", lambda e: e.reciprocal(out=stat[:, sidx + 2:sidx + 3], in_=stat[:, sidx + 1:sidx + 2]),
                 reads=[T_stat], writes=[T_stat])
            P.op("dve", lambda e: e.tensor_scalar(out=xn, in0=xt, scalar1=stat[:, sidx + 2:sidx + 3], scalar2=None, op0=ALU.mult),
                 reads=[T_xt, T_stat], writes=[T_xn])
            for g4 in range(4):
                pi = ps_ids[g4 % len(ps_ids)]
                for q in range(4):
                    dc = g4 * 4 + q
                    P.op("pe", (lambda e, pi=pi, q=q, dc=dc: e.transpose(out=psb[pi][:, q * 128:(q + 1) * 128],
                                                                           in_=xn[:, dc * 128:(dc + 1) * 128], identity=ident)),
                         reads=[T_xn, T_cst], writes=[Tps[pi]])
                for q in range(4):
                    dc = g4 * 4 + q
                    P.op("act", (lambda e, pi=pi, q=q, dc=dc: e.activation(out=dst_fn(dc), in_=psb[pi][:, q * 128:(q + 1) * 128],
                                                                             func=AF.Identity, scale=A_ap[:, dc:dc + 1], bias=B_ap[:, dc:dc + 1])),
                         reads=[Tps[pi], T_AB], writes=[T_dst])

        xtb = [pf(i * 2048, 2048) for i in range(2)]
        T_xtb = [T("xt0"), T("xt1")]
        xnb = [pf((2 + i) * 2048, 2048) for i in range(2)]
        T_xnb = [T("xn0"), T("xn1")]
        junkB = pb(8192 + 64, 2048)
        T_junk = T("junk")
        statB = pf(8192, 64)
        T_stat = T("stat")
        for j in range(NT_ALL):
            src = xl[j * 128:(j + 1) * 128, :] if j < 16 else ctxl[(j - 16) * 128:(j - 15) * 128, :]
            ai = 0 if j < 16 else 2
            col0 = j * 128
            norm_to_T(src, xtb[j % 2], T_xtb[j % 2], xnb[j % 2], T_xnb[j % 2], junkB, T_junk, statB, T_stat, (j % 2) * 3,
                      AB[:, ai, :], AB[:, ai + 1, :],
                      (lambda dc, col0=col0: hT[:, dc, col0:col0 + 128]), T_hT[j], [1 + (j % 2) * 2, 2 + (j % 2) * 2])
        P.barrier()

    if stage == 1:
        dbg_o = nc.dram_tensor("dbg", [128, NDC * (SEQ + CTX)], BF16, kind="ExternalOutput").ap()
        T_all = T("dbgall")
        d = P.dma("sp", dbg_o, bv(HT0 * 2, NDC * (SEQ + CTX)), reads=T_hT, owner=T_all, store=True)
        final_waits.append(d)
        with nc.allow_low_precision("bf16"):
            P.finalize(final_waits)
        P.close()
        es.close()
        return nc

    rowsbig = din("rowsbig", [128, 4096])
    wgate = din("wgate", [128, 16 * 32])
    wmh0 = din("wmh0", [8, 128, 16 * 256])
    wmh1 = din("wmh1", [8, 128, 16 * 512])
    waq = din("waq", [4, 128, 16 * 512])
    waqp = din("waqp", [4, 128, 16 * 512])
    wak = din("wak", [4, 128, 16 * 512])
    ropec = din("ropec", [128, 1152])
    ropes = din("ropes", [128, 1152])
    wz = din("wz", [16, 128, 4 * 16 * 128])
    wout = din("wout", [4, 128, 16 * 512])
    wr = din("wr", [128, 16 * 72])
    wg_e = din("wg_e", [64, D, 1024])
    wu_e = din("wu_e", [64, D, 1024])
    wd_e = din("wd_e", [64, 1024, D])
    WB0 = 0
    wbuf = [pb(WB0 + i * 4096, 8192).rearrange("p (a b) -> p a b", b=512) for i in range(2)]
    T_wbuf = [T("wbuf0"), T("wbuf1")]
    wslot = [0]

    def load_w(src_ap, ncols):
        i = wslot[0] % 2
        wslot[0] += 1
        view = pb(WB0 + i * 4096, 16 * ncols).rearrange("p (a b) -> p a b", b=ncols)
        P.dma("pool", view, src_ap.rearrange("p (a b) -> p a b", b=ncols), writes=[T_wbuf[i]], owner=T_wbuf[i])
        return view, T_wbuf[i]

    FO = 8192
    o = [FO]

    def af(n):
        r = pf(o[0], n)
        o[0] += n
        return r

    def ab(n):
        n2 = (n + 1) // 2
        r = pb(o[0], n)
        o[0] += n2
        return r

    G = af(NT_ALL * 32).rearrange("p (a b) -> p a b", b=32)
    SPt = af(NT_ALL * 16).rearrange("p (a b) -> p a b", b=16)
    CSP = af(NT_ALL * 32).rearrange("p (a b) -> p a b", b=32)
    At = af(NT_ALL * 16).rearrange("p (a b) -> p a b", b=16)
    AM = af(NT_ALL * 16).rearrange("p (a b) -> p a b", b=16)
    Mt = af(NT_ALL * 16).rearrange("p (a b) -> p a b", b=16)
    WC = af(NT_ALL * 16).rearrange("p (a b) -> p a b", b=16)
    WS = af(NT_ALL * 16).rearrange("p (a b) -> p a b", b=16)
    ET = af(NT_ALL * 16).rearrange("p (a b) -> p a b", b=16)
    gtmp = af(64)
    mprev = af(16)
    hacc = af(8 * 256).rearrange("p (a b) -> p a b", b=256)
    Cn = af(264)
    Cw = af(264)
    hmo = af(256)
    hst = af(16)
    mng = af(2048)
    T_G, T_SP, T_CSP, T_A, T_AM, T_M, T_WC, T_WS, T_ET = [T(n) for n in "G SP CSP A AM M WC WS ET".split()]
    T_gtmp, T_mprev, T_Cn, T_Cw, T_hmo, T_hst, T_mng = [T(n) for n in "gtmp mprev Cn Cw hmo hst mng".split()]
    T_hacc = [T("hacc%d" % j) for j in range(8)]
    ktok = ab(NT_ALL * 128).rearrange("p (a b) -> p a b", b=128)
    vext = ab(NT_ALL * 258).rearrange("p (a b) -> p a b", b=258)
    qT = ab(OWN)
    kT = ab(OWN)
    og = ab(8 * 256).rearrange("p (a b) -> p a b", b=256)
    Cwb = ab(264)
    PT = ab(128)
    kp = ab(128)
    zst = ab(2 * OWN).rearrange("p (a b) -> p a b", b=OWN)
    T_ktok = [T("ktok%d" % j) for j in range(NT_ALL)]
    T_vext = [T("vext%d" % j) for j in range(NT_ALL)]
    T_qT, T_kT, T_Cwb, T_PT, T_kp, T_zst = [T(n) for n in "qT kT Cwb PT kp zst".split()]
    T_og = [T("og%d" % j) for j in range(8)]
    MIX_END = o[0]

    P.dma("sp", mng, rowsbig[:, 0:2048], writes=[T_mng], owner=T_mng)
    P.op("dve", lambda e: e.memset(vext[:, :, 256:258], 1.0), writes=T_vext)

    wg_v, T_wg = load_w(wgate, 32)
    for j in range(NT_ALL):
        c0 = j * 128
        for dc in range(NDC):
            P.op("pe", (lambda e, dc=dc, c0=c0: e.matmul(psb[0][:, 0:32], lhsT=hT[:, dc, c0:c0 + 128], rhs=wg_v[:, dc, :],
                                                        start=(dc == 0), stop=(dc == NDC - 1))),
                 reads=[T_hT[j], T_wg], writes=[Tps[0]])
        P.op("dve", (lambda e, j=j: e.tensor_tensor(out=G[:, j, :], in0=psb[0][:, 0:32], in1=rows_sb[:, 0:32], op=ALU.add)),
             reads=[Tps[0], T_rows], writes=[T_G])
        P.op("act", (lambda e, j=j: e.activation(out=gtmp[:, 0:16], in_=G[:, j, 16:32], func=AF.Exp, scale=-1.0)),
             reads=[T_G], writes=[T_gtmp])
        P.op("act", (lambda e, j=j: e.activation(out=SPt[:, j, :], in_=gtmp[:, 0:16], func=AF.Ln, bias=ones[:, 0:1], scale=1.0)),
             reads=[T_gtmp, T_cst], writes=[T_SP])
        P.op("pe", (lambda e, j=j: e.matmul(psb[1][:, 0:8], lhsT=triF, rhs=SPt[:, j, 0:8], start=True, stop=True)),
             reads=[T_SP, T_cst], writes=[Tps[1]])
        P.op("pe", (lambda e, j=j: e.matmul(psb[1][:, 8:16], lhsT=triB, rhs=SPt[:, j, 8:16], start=True, stop=True)),
             reads=[T_SP, T_cst], writes=[Tps[1]])
        P.op("pe", (lambda e, j=j: e.matmul(psb[1][:, 16:32], lhsT=ones, rhs=SPt[:, j, 0:16], start=True, stop=True)),
             reads=[T_SP, T_cst], writes=[Tps[1]])
        P.op("dve", (lambda e, j=j: e.tensor_copy(out=CSP[:, j, :], in_=psb[1][:, 0:32])), reads=[Tps[1]], writes=[T_CSP])
        P.op("dve", (lambda e, j=j: e.tensor_tensor(out=At[:, j, :], in0=G[:, j, 0:16], in1=CSP[:, j, 0:16], op=ALU.add)),
             reads=[T_G, T_CSP], writes=[T_A])
        P.op("pe", (lambda e, j=j: e.transpose(out=psb[2][0:16, 0:128], in_=At[:, j, :], identity=ident)),
             reads=[T_A, T_cst], writes=[Tps[2]])
        P.op("dve", (lambda e: e.reduce_max(out=gtmp[0:16, 16:17], in_=psb[2][0:16, 0:128], axis=AX.X)),
             reads=[Tps[2]], writes=[T_gtmp])
        P.op("dve", (lambda e: e.tensor_scalar(out=gtmp[0:16, 32:48], in0=ident[0:16, 0:16], scalar1=gtmp[0:16, 16:17], scalar2=None, op0=ALU.mult)),
             reads=[T_gtmp, T_cst], writes=[T_gtmp])
        P.op("pe", (lambda e: e.matmul(psb[3][:, 0:16], lhsT=ones[0:16, :], rhs=gtmp[0:16, 32:48], start=True, stop=True)),
             reads=[T_gtmp, T_cst], writes=[Tps[3]])
        P.op("dve", (lambda e, j=j: e.tensor_copy(out=AM[:, j, :], in_=psb[3][:, 0:16])), reads=[Tps[3]], writes=[T_AM])

    if stage == 21:
        return finish([])
    steps = {0: [16, 17] + list(range(8)), 1: [17, 16] + list(range(15, -1, -1))}
    P.op("dve", lambda e: e.memset(mprev, 0.0), writes=[T_mprev])
    for d in (0, 1):
        cs = slice(d * 8, (d + 1) * 8)
        for j in steps[d]:
            P.op("dve", (lambda e, j=j, cs=cs: e.tensor_tensor(out=Mt[:, j, cs], in0=mprev[:, cs], in1=AM[:, j, cs], op=ALU.max)),
                 reads=[T_mprev, T_AM], writes=[T_M])
            P.op("dve", (lambda e, j=j, cs=cs: e.tensor_tensor(out=WC[:, j, cs], in0=mprev[:, cs], in1=Mt[:, j, cs], op=ALU.subtract)),
                 reads=[T_mprev, T_M], writes=[T_WC])
            P.op("dve", (lambda e, j=j, cs=cs, d=d: e.tensor_tensor(out=mprev[:, cs], in0=Mt[:, j, cs], in1=CSP[:, j, 16 + d * 8:24 + d * 8], op=ALU.subtract)),
                 reads=[T_M, T_CSP], writes=[T_mprev])
    P.op("act", lambda e: e.activation(out=WC, in_=WC, func=AF.Exp), reads=[T_WC], writes=[T_WC])
    P.op("dve", lambda e: e.tensor_tensor(out=WS, in0=At, in1=Mt, op=ALU.subtract), reads=[T_A, T_M], writes=[T_WS])
    P.op("act", lambda e: e.activation(out=WS, in_=WS, func=AF.Exp, bias=small[:, 1:2], scale=1.0), reads=[T_WS, T_small], writes=[T_WS])
    P.op("dve", lambda e: e.tensor_tensor(out=ET, in0=CSP[:, :, 0:16], in1=Mt, op=ALU.subtract), reads=[T_CSP, T_M], writes=[T_ET])
    P.op("act", lambda e: e.activation(out=ET, in_=ET, func=AF.Exp), reads=[T_ET], writes=[T_ET])

    if stage == 22:
        return finish([])
    T_zsrc = T("zsrc")
    z_stores = []
    for h in range(nmh):
        w0, T_w0 = load_w(wmh0[h], 256)
        for which, dstq, T_dq in ((0, qT, T_qT), (1, kT, T_kT)):
            for half in range(2):
                pi = 4 + half
                for dc in range(NDC):
                    P.op("pe", (lambda e, dc=dc, half=half, which=which, pi=pi, w0=w0: e.matmul(
                        psb[pi][:, :], lhsT=w0[:, dc, which * 128:(which + 1) * 128], rhs=hT[:, dc, half * 512:(half + 1) * 512],
                        start=(dc == 0), stop=(dc == NDC - 1))), reads=T_hT[half * 4:half * 4 + 4] + [T_w0], writes=[Tps[pi]])
                P.op("act", (lambda e, half=half, pi=pi, dstq=dstq: e.activation(out=dstq[:, half * 512:(half + 1) * 512], in_=psb[pi][:, :], func=AF.Identity)),
                     reads=[Tps[pi]], writes=[T_dq])
        if sub == 1:
            return finish([])
        for j in range(NT_ALL):
            pi = 6 + (j % 2)
            c0 = j * 128
            for dc in range(NDC):
                P.op("pe", (lambda e, dc=dc, c0=c0, pi=pi, w0=w0: e.matmul(psb[pi][:, 0:128], lhsT=hT[:, dc, c0:c0 + 128], rhs=w0[:, dc, 128:256],
                                                                  start=(dc == 0), stop=(dc == NDC - 1))),
                     reads=[T_hT[j], T_w0], writes=[Tps[pi]])
            P.op("act", (lambda e, j=j, pi=pi: e.activation(out=ktok[:, j, :], in_=psb[pi][:, 0:128], func=AF.Identity)),
                 reads=[Tps[pi]], writes=[T_ktok[j]])
        if sub == 2:
            return finish([])
        w1, T_w1 = load_w(wmh1[h], 512)
        for j in range(NT_ALL):
            pi = 6 + (j % 2)
            c0 = j * 128
            ncol = 512 if j < 8 else 256
            for dc in range(NDC):
                P.op("pe", (lambda e, dc=dc, c0=c0, pi=pi, ncol=ncol, w1=w1: e.matmul(psb[pi][:, 0:ncol], lhsT=hT[:, dc, c0:c0 + 128], rhs=w1[:, dc, 0:ncol],
                                                                              start=(dc == 0), stop=(dc == NDC - 1))),
                     reads=[T_hT[j], T_w1], writes=[Tps[pi]])
            P.op("dve", (lambda e, j=j, pi=pi: e.tensor_copy(out=vext[:, j, 0:256], in_=psb[pi][:, 0:256])),
                 reads=[Tps[pi]], writes=[T_vext[j]])
            if j < 8:
                P.op("act", (lambda e, j=j, pi=pi: e.activation(out=og[:, j, :], in_=psb[pi][:, 256:512], func=AF.Sigmoid)),
                     reads=[Tps[pi]], writes=[T_og[j]])
        if stage == 23:
            return finish([])
        for d in (0, 1):
            col = d * 8 + h
            msk = triF if d == 0 else triB
            P.op("dve", lambda e: e.memset(Cn, 0.0), writes=[T_Cn])
            nst = len(steps[d])
            for si, j in enumerate(steps[d]):
                full = j < 8
                c0 = j * 128
                P.op("dve", (lambda e, j=j, col=col: e.tensor_scalar(out=Cw[:, 0:257], in0=Cn[:, 0:257], scalar1=WC[:, j, col:col + 1], scalar2=None, op0=ALU.mult)),
                     reads=[T_Cn, T_WC], writes=[T_Cw])
                if full:
                    P.op("act", lambda e: e.activation(out=Cwb[:, 0:257], in_=Cw[:, 0:257], func=AF.Identity), reads=[T_Cw], writes=[T_Cwb])
                    P.op("pe", (lambda e, c0=c0: e.matmul(psb[0][:, 0:128], lhsT=kT[:, c0:c0 + 128], rhs=qT[:, c0:c0 + 128], start=True, stop=True)),
                         reads=[T_kT, T_qT], writes=[Tps[0]])
                    P.op("dve", (lambda e, j=j, col=col, msk=msk: e.scalar_tensor_tensor(out=PT, in0=psb[0][:, 0:128], scalar=WS[:, j, col:col + 1], in1=msk,
                                                                                         op0=ALU.mult, op1=ALU.mult)),
                         reads=[Tps[0], T_WS, T_cst], writes=[T_PT])
                    P.op("pe", (lambda e, j=j: e.matmul(psb[1][:, 0:257], lhsT=PT, rhs=vext[:, j, 0:257], start=True, stop=False)),
                         reads=[T_PT, T_vext[j]], writes=[Tps[1]])
                    P.op("pe", (lambda e, c0=c0: e.matmul(psb[1][:, 0:257], lhsT=qT[:, c0:c0 + 128], rhs=Cwb[:, 0:257], start=False, stop=True)),
                         reads=[T_qT, T_Cwb], writes=[Tps[1]])
                    P.op("act", lambda e: e.activation(out=hst[:, 2:3], in_=psb[1][:, 256:257], func=AF.Abs), reads=[Tps[1]], writes=[T_hst])
                    P.op("dve", (lambda e, j=j, col=col: e.tensor_tensor(out=hst[:, 0:1], in0=hst[:, 2:3], in1=ET[:, j, col:col + 1], op=ALU.max)),
                         reads=[T_hst, T_ET], writes=[T_hst])
                    P.op("dve", lambda e: e.reciprocal(out=hst[:, 1:2], in_=hst[:, 0:1]), reads=[T_hst], writes=[T_hst])
                    if d == 0:
                        P.op("act", (lambda e, j=j: e.activation(out=hacc[:, j, :], in_=psb[1][:, 0:256], func=AF.Identity, scale=hst[:, 1:2])),
                             reads=[Tps[1], T_hst], writes=[T_hacc[j]])
                    else:
                        P.op("dve", (lambda e, j=j: e.scalar_tensor_tensor(out=hacc[:, j, :], in0=psb[1][:, 0:256], scalar=hst[:, 1:2], in1=hacc[:, j, :],
                                                                           op0=ALU.mult, op1=ALU.add)),
                             reads=[Tps[1], T_hst], writes=[T_hacc[j]])
                if si == nst - 1:
                    continue
                P.op("act", (lambda e, j=j, col=col: e.activation(out=kp, in_=ktok[:, j, :], func=AF.Identity, scale=WS[:, j, col:col + 1])),
                     reads=[T_ktok[j], T_WS], writes=[T_kp])
                P.op("pe", (lambda e, j=j: e.matmul(psb[2][:, 0:257], lhsT=kp, rhs=vext[:, j, 0:257], start=True, stop=True)),
                     reads=[T_kp, T_vext[j]], writes=[Tps[2]])
                P.op("dve", lambda e: e.tensor_tensor(out=Cn[:, 0:257], in0=psb[2][:, 0:257], in1=Cw[:, 0:257], op=ALU.add),
                     reads=[Tps[2], T_Cw], writes=[T_Cn])
        for j in range(8):
            P.op("act", (lambda e, j=j: e.activation(out=hmo, in_=hacc[:, j, :], func=AF.Square, accum_out=hst[:, 4:5])),
                 reads=[T_hacc[j]], writes=[T_hmo, T_hst])
            P.op("act", lambda e: e.activation(out=hst[:, 5:6], in_=hst[:, 4:5], func=AF.Sqrt, bias=small[:, 0:1], scale=1.0 / 256),
                 reads=[T_hst, T_small], writes=[T_hst])
            P.op("dve", lambda e: e.reciprocal(out=hst[:, 6:7], in_=hst[:, 5:6]), reads=[T_hst], writes=[T_hst])
            P.op("dve", (lambda e, j=j, h=h: e.scalar_tensor_tensor(out=hmo, in0=hacc[:, j, :], scalar=hst[:, 6:7], in1=mng[:, h * 256:(h + 1) * 256],
                                                                    op0=ALU.mult, op1=ALU.mult)),
                 reads=[T_hacc[j], T_hst, T_mng], writes=[T_hmo])
            P.op("dve", (lambda e, j=j: e.tensor_tensor(out=hmo, in0=hmo, in1=og[:, j, :], op=ALU.mult)), reads=[T_hmo, T_og[j]], writes=[T_hmo])
            for i2 in range(2):
                P.op("pe", (lambda e, i2=i2: e.transpose(out=psb[3][:, i2 * 128:(i2 + 1) * 128], in_=hmo[:, i2 * 128:(i2 + 1) * 128], identity=ident)),
                     reads=[T_hmo, T_cst], writes=[Tps[3]])
            P.op("act", (lambda e, j=j: e.activation(out=zst[:, :, j * 128:(j + 1) * 128], in_=psb[3][:, 0:256].rearrange("p (a b) -> p a b", b=128), func=AF.Identity)),
                 reads=[Tps[3]], writes=[T_zst])
        for i2 in range(2):
            z_stores.append(P.dma("sp", zsrc[2 * h + i2], zst[:, i2, :], reads=[T_zst], owner=T_zst, store=True))
    P.barrier()
    if stage == 2:
        return finish(z_stores)
    o[0] = FO
    rC = af(1152)
    rS = af(1152)
    sc = af(640)
    Pm = af(640)
    rt1 = af(512)
    rt2 = af(512)
    ast = af(16)
    T_rC, T_sc, T_Pm, T_rt1, T_rt2, T_ast = [T(n) for n in "rC sc Pm rt1 rt2 ast".split()]
    qTa = ab(4 * OWN).rearrange("p (a b) -> p a b", b=OWN)
    kTa = ab(1152 + CTX)
    vta = ab(11 * 128).rearrange("p (a b) -> p a b", b=128)
    PnT = ab(640).rearrange("p (a b) -> p a b", b=128)
    aost = ab(OWN)
    T_qTa = [T("qTa%d" % i) for i in range(4)]
    T_kTa, T_PnT, T_aost = T("kTa"), T("PnT"), T("aost")
    T_vta = [T("vta%d" % i) for i in range(11)]
    assert o[0] <= 32520
    P.dma("sp", rC, ropec, writes=[T_rC], owner=T_rC)
    P.dma("sp", rS, ropes, writes=[T_rC], owner=T_rC)
    SCALE = 128.0 ** -0.5

    def rope_proj(wv, Tw, wc0, wpc0, tok0, ntok, dst, T_dst, rope=True):
        return rope_proj2(wv, Tw, wv, Tw, wc0, tok0, ntok, dst, T_dst, rope=rope, wpc0=wpc0)

    def rope_proj2(wv, Tw, wpv, Twp, wc0, tok0, ntok, dst, T_dst, rope=True, wpc0=None):
        if wpc0 is None:
            wpc0 = wc0
        tiles = [T_hT[t] for t in range(tok0 // 128, (tok0 + ntok + 127) // 128)]
        for dc in range(NDC):
            P.op("pe", (lambda e, dc=dc: e.matmul(psb[4][:, 0:ntok], lhsT=wv[:, dc, wc0:wc0 + 128], rhs=hT[:, dc, tok0:tok0 + ntok],
                                                  start=(dc == 0), stop=(dc == NDC - 1))), reads=tiles + [Tw], writes=[Tps[4]])
        if not rope:
            P.op("act", lambda e: e.activation(out=dst, in_=psb[4][:, 0:ntok], func=AF.Identity), reads=[Tps[4]], writes=[T_dst])
            return
        for dc in range(NDC):
            P.op("pe", (lambda e, dc=dc: e.matmul(psb[5][:, 0:ntok], lhsT=wpv[:, dc, wpc0:wpc0 + 128], rhs=hT[:, dc, tok0:tok0 + ntok],
                                                  start=(dc == 0), stop=(dc == NDC - 1))), reads=tiles + [Twp], writes=[Tps[5]])
        P.op("dve", lambda e: e.tensor_tensor(out=rt1[:, 0:ntok], in0=psb[4][:, 0:ntok], in1=rC[:, tok0:tok0 + ntok], op=ALU.mult),
             reads=[Tps[4], T_rC], writes=[T_rt1])
        P.op("dve", lambda e: e.tensor_tensor(out=rt2[:, 0:ntok], in0=psb[5][:, 0:ntok], in1=rS[:, tok0:tok0 + ntok], op=ALU.mult),
             reads=[Tps[5], T_rC], writes=[T_rt2])
        P.op("dve", lambda e: e.tensor_tensor(out=dst, in0=rt1[:, 0:ntok], in1=rt2[:, 0:ntok], op=ALU.add),
             reads=[T_rt1, T_rt2], writes=[T_dst])

    for g in range(nag):
        wk, T_wk = load_w(wak[g], 512)
        for (t0, nt) in ((0, 512), (512, 512), (1024, 128)):
            rope_proj(wk, T_wk, 0, 128, t0, nt, kTa[:, t0:t0 + nt], T_kTa)
        rope_proj(wk, T_wk, 0, 0, SEQ, CTX, kTa[:, 1152:1152 + CTX], T_kTa, rope=False)
        for vi, j in enumerate(list(range(9)) + [16, 17]):
            c0 = j * 128
            for dc in range(NDC):
                P.op("pe", (lambda e, dc=dc, c0=c0, wk=wk: e.matmul(psb[6][:, 0:128], lhsT=hT[:, dc, c0:c0 + 128], rhs=wk[:, dc, 256:384],
                                                            start=(dc == 0), stop=(dc == NDC - 1))), reads=[T_hT[j], T_wk], writes=[Tps[6]])
            P.op("act", (lambda e, vi=vi: e.activation(out=vta[:, vi, :], in_=psb[6][:, 0:128], func=AF.Identity)), reads=[Tps[6]], writes=[T_vta[vi]])
        wq, T_wq = load_w(waq[g], 512)
        wqp, T_wqp = load_w(waqp[g], 512)
        for hh in range(4):
            for half in range(2):
                rope_proj2(wq, T_wq, wqp, T_wqp, hh * 128, half * 512, 512, qTa[:, hh, half * 512:(half + 1) * 512], T_qTa[hh])
        for hh in range(4):
            head = g * 4 + hh
            sk = rows_sb[:, 32 + head:33 + head]
            for n in range(8):
                q_ap = qTa[:, hh, n * 128:(n + 1) * 128]
                if n == 0:
                    P.op("pe", lambda e, q_ap=q_ap: e.matmul(psb[0][:, 0:128], lhsT=q_ap, rhs=kTa[:, 0:128], start=True, stop=True),
                         reads=[T_qTa[hh], T_kTa], writes=[Tps[0]])
                    P.op("pe", lambda e, q_ap=q_ap: e.matmul(psb[0][:, 128:384], lhsT=q_ap, rhs=kTa[:, 0:256], start=True, stop=True),
                         reads=[T_qTa[hh], T_kTa], writes=[Tps[0]])
                else:
                    P.op("pe", lambda e, q_ap=q_ap, n=n: e.matmul(psb[0][:, 0:384], lhsT=q_ap, rhs=kTa[:, (n - 1) * 128:(n + 2) * 128], start=True, stop=True),
                         reads=[T_qTa[hh], T_kTa], writes=[Tps[0]])
                P.op("pe", lambda e, q_ap=q_ap: e.matmul(psb[1][:, 0:256], lhsT=q_ap, rhs=kTa[:, 1152:1152 + CTX], start=True, stop=True),
                     reads=[T_qTa[hh], T_kTa], writes=[Tps[1]])
                mk = mask3f if n == 0 else mask3
                P.op("dve", lambda e, mk=mk: e.scalar_tensor_tensor(out=sc[:, 0:384], in0=psb[0][:, 0:384], scalar=SCALE, in1=mk, op0=ALU.mult, op1=ALU.add),
                     reads=[Tps[0], T_cst], writes=[T_sc])
                P.op("act", lambda e: e.activation(out=sc[:, 384:640], in_=psb[1][:, 0:256], func=AF.Identity, scale=SCALE), reads=[Tps[1]], writes=[T_sc])
                P.op("dve", lambda e: e.reduce_max(out=ast[:, 0:1], in_=sc, axis=AX.X), reads=[T_sc], writes=[T_ast])
                P.op("dve", lambda e, sk=sk: e.tensor_tensor(out=ast[:, 1:2], in0=ast[:, 0:1], in1=sk, op=ALU.max), reads=[T_ast, T_rows], writes=[T_ast])
                P.op("dve", lambda e: e.tensor_scalar(out=ast[:, 2:3], in0=ast[:, 1:2], scalar1=-1.0, scalar2=None, op0=ALU.mult), reads=[T_ast], writes=[T_ast])
                P.op("act", lambda e: e.activation(out=Pm, in_=sc, func=AF.Exp, bias=ast[:, 2:3], scale=1.0, accum_out=ast[:, 3:4]),
                     reads=[T_sc, T_ast], writes=[T_Pm, T_ast])
                P.op("act", lambda e, sk=sk: e.activation(out=ast[:, 4:5], in_=sk, func=AF.Exp, bias=ast[:, 2:3], scale=1.0), reads=[T_rows, T_ast], writes=[T_ast])
                P.op("dve", lambda e: e.tensor_tensor(out=ast[:, 5:6], in0=ast[:, 3:4], in1=ast[:, 4:5], op=ALU.add), reads=[T_ast], writes=[T_ast])
                P.op("dve", lambda e: e.reciprocal(out=ast[:, 6:7], in_=ast[:, 5:6]), reads=[T_ast], writes=[T_ast])
                P.op("dve", lambda e: e.tensor_scalar(out=Pm, in0=Pm, scalar1=ast[:, 6:7], scalar2=None, op0=ALU.mult), reads=[T_Pm, T_ast], writes=[T_Pm])
                for kb in range(5):
                    pi, off = (2, kb * 128) if kb < 4 else (3, 0)
                    P.op("pe", lambda e, kb=kb, pi=pi, off=off: e.transpose(out=psb[pi][:, off:off + 128], in_=Pm[:, kb * 128:(kb + 1) * 128], identity=ident),
                         reads=[T_Pm, T_cst], writes=[Tps[pi]])
                P.op("act", lambda e: e.activation(out=PnT[:, 0:4, :], in_=psb[2][:, :].rearrange("p (a b) -> p a b", b=128), func=AF.Identity),
                     reads=[Tps[2]], writes=[T_PnT])
                P.op("dve", lambda e: e.tensor_copy(out=PnT[:, 4, :], in_=psb[3][:, 0:128]), reads=[Tps[3]], writes=[T_PnT])
                vidx = [max(n - 1, 0), n, n + 1, 9, 10]
                for kb in range(5):
                    P.op("pe", lambda e, kb=kb, vi=vidx[kb]: e.matmul(psb[7][:, 0:128], lhsT=vta[:, vi, :], rhs=PnT[:, kb, :], start=(kb == 0), stop=(kb == 4)),
                         reads=[T_vta[vidx[kb]], T_PnT], writes=[Tps[7]])
                P.op("act", lambda e, n=n: e.activation(out=aost[:, n * 128:(n + 1) * 128], in_=psb[7][:, 0:128], func=AF.Identity), reads=[Tps[7]], writes=[T_aost])
            z_stores.append(P.dma("sp", zsrc[16 + head], aost, reads=[T_aost], owner=T_aost, store=True))
    P.barrier()
    if stage == 3:
        return finish(z_stores)
    C_H2T, C_WT, C_G2B, C_G1B, C_END = 0, 8192, 8704, 10752, 12800
    o[0] = C_END
    zT = ab(16 * OWN).rearrange("p (a b) -> p a b", b=OWN)
    T_zT = T("zT")
    zin = ab(32 * 512).rearrange("p (a b) -> p a b", b=512)
    T_zin = T("zin")
    o[0] = 0
    wzb = [ab(8192).rearrange("p (k a b) -> p k a b", k=4, a=16, b=128) for _ in range(2)]
    T_wzb = [T("wzb0"), T("wzb1")]
    sgm = af(512)
    sga = af(512)
    T_sgm, T_sga = T("sgm"), T("sga")
    assert o[0] <= 32520
    for half in range(2):
        hs = slice(half * 512, (half + 1) * 512)
        P.dma("sp", zin, zsrc[:, :, hs].rearrange("k p t -> p k t"), writes=[T_zin], owner=T_zin)
        for j in range(16):
            wv = wzb[j % 2]
            tw = T_wzb[j % 2]
            P.dma("pool", wv, wz[j].rearrange("p (k a b) -> p k a b", k=4, a=16, b=128), writes=[tw], owner=tw)
            for k4, (src_is_h, pi) in enumerate(((False, 0), (False, 1), (True, 2), (True, 3))):
                for dc in range(NDC):
                    if src_is_h:
                        rhs = hT[:, dc, hs]
                        rd = T_hT[half * 4:half * 4 + 4]
                    else:
                        rhs = zin[:, k4 * 16 + dc, :]
                        rd = [T_zin]
                    P.op("pe", (lambda e, wv=wv, k4=k4, dc=dc, pi=pi, rhs=rhs: e.matmul(psb[pi][:, :], lhsT=wv[:, k4, dc, :], rhs=rhs,
                                                                                       start=(dc == 0), stop=(dc == NDC - 1))),
                         reads=rd + [tw], writes=[Tps[pi]])
            P.op("act", lambda e: e.activation(out=sgm, in_=psb[2][:, :], func=AF.Sigmoid), reads=[Tps[2]], writes=[T_sgm])
            P.op("act", lambda e: e.activation(out=sga, in_=psb[3][:, :], func=AF.Sigmoid), reads=[Tps[3]], writes=[T_sga])
            P.op("dve", lambda e: e.tensor_tensor(out=sgm, in0=psb[0][:, :], in1=sgm, op=ALU.mult), reads=[Tps[0], T_sgm], writes=[T_sgm])
            P.op("dve", lambda e: e.tensor_tensor(out=sga, in0=psb[1][:, :], in1=sga, op=ALU.mult), reads=[Tps[1], T_sga], writes=[T_sga])
            P.op("dve", (lambda e, j=j, hs=hs: e.tensor_tensor(out=zT[:, j, hs], in0=sgm, in1=sga, op=ALU.add)), reads=[T_sgm, T_sga], writes=[T_zT])
    P.barrier()

    if stage == 4:
        return finish(z_stores)
    g2b = pf(C_G2B, 2048)
    g1b = pf(C_G1B, 2048)
    T_gb = T("gb")
    x1 = fv(HT0, 8 * 2048).rearrange("p (a b) -> p a b", b=2048)
    T_x1 = [T("x1_%d" % n) for n in range(8)]
    o[0] = C_END + 8192
    wob = [ab(8192).rearrange("p (a b) -> p a b", b=512) for _ in range(2)]
    T_wob = [T("wob0"), T("wob1")]
    xp = [af(512) for _ in range(2)]
    T_xp = [T("xp0"), T("xp1")]
    dgt = af(128)
    T_dgt = T("dgt")
    assert o[0] <= 32520
    for which, dstb in ((6, g1b), (7, g2b)):
        for dc in range(NDC):
            P.op("dve", (lambda e, which=which, dc=dc: e.tensor_scalar(out=dgt, in0=ident, scalar1=AB[:, which, dc:dc + 1], scalar2=None, op0=ALU.mult)),
                 reads=[T_AB, T_cst], writes=[T_dgt])
            P.op("pe", lambda e: e.matmul(psb[4][:, 0:128], lhsT=ones, rhs=dgt, start=True, stop=True), reads=[T_dgt, T_cst], writes=[Tps[4]])
            P.op("act", (lambda e, dstb=dstb, dc=dc: e.activation(out=dstb[:, dc * 128:(dc + 1) * 128], in_=psb[4][:, 0:128], func=AF.Identity)),
                 reads=[Tps[4]], writes=[T_gb])
    cnt = 0
    for cb in range(4):
        wv = wob[cb % 2]
        tw = T_wob[cb % 2]
        P.dma("pool", wv, wout[cb].rearrange("p (a b) -> p a b", b=512), writes=[tw], owner=tw)
        for n in range(8):
            pi = 5 + (cnt % 2)
            xpi = xp[cnt % 2]
            txp = T_xp[cnt % 2]
            cnt += 1
            P.dma("sp", xpi, xl[n * 128:(n + 1) * 128, cb * 512:(cb + 1) * 512], writes=[txp], owner=txp)
            for k in range(16):
                P.op("pe", (lambda e, wv=wv, k=k, n=n, pi=pi: e.matmul(psb[pi][:, :], lhsT=zT[:, k, n * 128:(n + 1) * 128], rhs=wv[:, k, :],
                                                                      start=(k == 0), stop=(k == 15))), reads=[T_zT, tw], writes=[Tps[pi]])
            P.op("dve", (lambda e, pi=pi, cb=cb, n=n: e.tensor_tensor(out=x1[:, n, cb * 512:(cb + 1) * 512], in0=psb[pi][:, :], in1=g1b[:, cb * 512:(cb + 1) * 512], op=ALU.mult)),
                 reads=[Tps[pi], T_gb], writes=[T_x1[n]])
            P.op("dve", (lambda e, xpi=xpi, cb=cb, n=n: e.tensor_tensor(out=x1[:, n, cb * 512:(cb + 1) * 512], in0=x1[:, n, cb * 512:(cb + 1) * 512], in1=xpi, op=ALU.add)),
                 reads=[txp], writes=[T_x1[n]])
    P.barrier()

    h2T = pb(C_H2T, 16 * OWN).rearrange("p (a b) -> p a b", b=OWN)
    T_h2T = T("h2T")
    Wt = pf(C_WT, 512).rearrange("p (a b) -> p a b", b=64)
    T_Wt = T("Wt")
    o[0] = C_END
    xn2 = af(2048)
    h2f = af(2048).rearrange("p (a b) -> p a b", b=128)
    wr_sb = af(1152).rearrange("p (a b) -> p a b", b=72)
    junk2 = ab(2048)
    st2 = af(64)
    lg = af(72)
    rt = af(256)
    T_xn2, T_h2f, T_wr, T_junk2, T_st2, T_lg, T_rt = [T(n) for n in "xn2 h2f wr junk2 st2 lg rt".split()]
    assert o[0] <= 32520
    P.dma("sp", wr_sb, wr.rearrange("p (a b) -> p a b", b=72), writes=[T_wr], owner=T_wr)
    x1_stores = []
    for n in range(8):
        x1_stores.append(P.dma("sp", x1s[n * 128:(n + 1) * 128, :], x1[:, n, :], reads=[T_x1[n]], owner=T_x1[n], store=True))
        xt = x1[:, n, :]
        P.op("act", (lambda e, xt=xt: e.activation(out=junk2, in_=xt, func=AF.Square, accum_out=st2[:, 0:1])), reads=[T_x1[n]], writes=[T_junk2, T_st2])
        P.op("act", lambda e: e.activation(out=st2[:, 1:2], in_=st2[:, 0:1], func=AF.Sqrt, bias=small[:, 0:1], scale=1.0 / D), reads=[T_st2, T_small], writes=[T_st2])
        P.op("dve", lambda e: e.reciprocal(out=st2[:, 2:3], in_=st2[:, 1:2]), reads=[T_st2], writes=[T_st2])
        P.op("dve", (lambda e, xt=xt: e.tensor_scalar(out=xn2, in0=xt, scalar1=st2[:, 2:3], scalar2=None, op0=ALU.mult)), reads=[T_x1[n], T_st2], writes=[T_xn2])
        for g4 in range(4):
            pi = 1 + (g4 % 2)
            for q in range(4):
                dc = g4 * 4 + q
                P.op("pe", (lambda e, pi=pi, q=q, dc=dc: e.transpose(out=psb[pi][:, q * 128:(q + 1) * 128], in_=xn2[:, dc * 128:(dc + 1) * 128], identity=ident)),
                     reads=[T_xn2, T_cst], writes=[Tps[pi]])
            for q in range(4):
                dc = g4 * 4 + q
                P.op("act", (lambda e, pi=pi, q=q, dc=dc, n=n: e.activation(out=h2T[:, dc, n * 128:(n + 1) * 128], in_=psb[pi][:, q * 128:(q + 1) * 128],
                                                                             func=AF.Identity, scale=AB[:, 4, dc:dc + 1], bias=AB[:, 5, dc:dc + 1])),
                     reads=[Tps[pi], T_AB], writes=[T_h2T])
                P.op("act", (lambda e, pi=pi, q=q, dc=dc: e.activation(out=h2f[:, dc, :], in_=psb[pi][:, q * 128:(q + 1) * 128],
                                                                       func=AF.Identity, scale=AB[:, 4, dc:dc + 1], bias=AB[:, 5, dc:dc + 1])),
                     reads=[Tps[pi], T_AB], writes=[T_h2f])
        for dc in range(NDC):
            P.op("pe", (lambda e, dc=dc: e.matmul(psb[3][:, 0:72], lhsT=h2f[:, dc, :], rhs=wr_sb[:, dc, :], start=(dc == 0), stop=(dc == NDC - 1))),
                 reads=[T_h2f, T_wr], writes=[Tps[3]])
        P.op("dve", lambda e: e.tensor_tensor(out=lg, in0=psb[3][:, 0:72], in1=rows_sb[:, 48:120], op=ALU.add), reads=[Tps[3], T_rows], writes=[T_lg])
        R = [T_rt, T_lg]
        P.op("dve", lambda e: e.reduce_max(out=rt[:, 0:1], in_=lg[:, 0:8], axis=AX.X), reads=[T_lg], writes=[T_rt])
        P.op("dve", lambda e: e.tensor_scalar(out=rt[:, 8:16], in0=lg[:, 0:8], scalar1=rt[:, 0:1], scalar2=None, op0=ALU.is_equal), reads=R, writes=[T_rt])
        P.op("dve", lambda e: e.tensor_scalar(out=rt[:, 1:2], in0=rt[:, 0:1], scalar1=-1.0, scalar2=None, op0=ALU.mult), reads=R, writes=[T_rt])
        P.op("act", lambda e: e.activation(out=rt[:, 16:24], in_=lg[:, 0:8], func=AF.Exp, bias=rt[:, 1:2], scale=1.0, accum_out=rt[:, 2:3]), reads=R, writes=[T_rt])
        P.op("dve", lambda e: e.reciprocal(out=rt[:, 3:4], in_=rt[:, 2:3]), reads=R, writes=[T_rt])
        P.op("dve", lambda e: e.tensor_tensor(out=rt[:, 64:128].rearrange("p (g k) -> p g k", k=8), in0=lg[:, 8:72].rearrange("p (g k) -> p g k", k=8),
                                              in1=rt[:, 8:16].unsqueeze(2).to_broadcast([128, 8, 8]), op=ALU.mult), reads=R, writes=[T_rt])
        P.op("dve", lambda e: e.reduce_sum(out=rt[:, 24:32], in_=rt[:, 64:128].rearrange("p (g k) -> p k g", k=8), axis=AX.X), reads=R, writes=[T_rt])
        P.op("dve", lambda e: e.reduce_max(out=rt[:, 4:5], in_=rt[:, 24:32], axis=AX.X), reads=R, writes=[T_rt])
        P.op("dve", lambda e: e.tensor_scalar(out=rt[:, 32:40], in0=rt[:, 24:32], scalar1=rt[:, 4:5], scalar2=None, op0=ALU.is_equal), reads=R, writes=[T_rt])
        P.op("dve", lambda e: e.scalar_tensor_tensor(out=rt[:, 40:48], in0=rt[:, 32:40], scalar=-1e30, in1=rt[:, 24:32], op0=ALU.mult, op1=ALU.add), reads=R, writes=[T_rt])
        P.op("dve", lambda e: e.reduce_max(out=rt[:, 5:6], in_=rt[:, 40:48], axis=AX.X), reads=R, writes=[T_rt])
        P.op("dve", lambda e: e.tensor_scalar(out=rt[:, 48:56], in0=rt[:, 40:48], scalar1=rt[:, 5:6], scalar2=None, op0=ALU.is_equal), reads=R, writes=[T_rt])
        P.op("dve", lambda e: e.tensor_tensor(out=rt[:, 6:7], in0=rt[:, 4:5], in1=rt[:, 5:6], op=ALU.subtract), reads=R, writes=[T_rt])
        P.op("act", lambda e: e.activation(out=rt[:, 7:8], in_=rt[:, 6:7], func=AF.Sigmoid), reads=R, writes=[T_rt])
        P.op("dve", lambda e: e.tensor_tensor(out=rt[:, 56:57], in0=rt[:, 7:8], in1=rt[:, 3:4], op=ALU.mult), reads=R, writes=[T_rt])
        P.op("dve", lambda e: e.tensor_tensor(out=rt[:, 57:58], in0=rt[:, 3:4], in1=rt[:, 56:57], op=ALU.subtract), reads=R, writes=[T_rt])
        P.op("dve", lambda e: e.tensor_scalar(out=rt[:, 32:40], in0=rt[:, 32:40], scalar1=rt[:, 56:57], scalar2=None, op0=ALU.mult), reads=R, writes=[T_rt])
        P.op("dve", lambda e: e.scalar_tensor_tensor(out=rt[:, 32:40], in0=rt[:, 48:56], scalar=rt[:, 57:58], in1=rt[:, 32:40], op0=ALU.mult, op1=ALU.add), reads=R, writes=[T_rt])
        P.op("dve", (lambda e, n=n: e.tensor_tensor(out=Wt[:, n, :].rearrange("p (g k) -> p g k", k=8),
                                                    in0=rt[:, 8:16].unsqueeze(2).to_broadcast([128, 8, 8]),
                                                    in1=rt[:, 32:40].unsqueeze(1).to_broadcast([128, 8, 8]), op=ALU.mult)), reads=R, writes=[T_Wt])
    P.barrier()
    if stage == 5:
        return finish(z_stores + x1_stores)
    yacc = fv(HT0, 8 * 2048).rearrange("p (a b) -> p a b", b=2048)
    T_yacc = [T("yacc%d" % n) for n in range(8)]
    o[0] = C_END
    gub = [ab(8192).rearrange("p (k a b) -> p k a b", k=2, a=16, b=256) for _ in range(2)]
    T_gub = [T("gub0"), T("gub1")]
    dwb = [ab(4096).rearrange("p (a b) -> p a b", b=512) for _ in range(2)]
    T_dwb = [T("dwb0"), T("dwb1")]
    actT = ab(8 * OWN).rearrange("p (a b) -> p a b", b=OWN)
    T_actT = T("actT")
    sgt = [af(512) for _ in range(2)]
    T_sgt = [T("sgt0"), T("sgt1")]
    assert o[0] <= 32520
    for n in range(8):
        P.op("dve", (lambda e, n=n: e.memset(yacc[:, n, :], 0.0)), writes=[T_yacc[n]])
    gi = 0
    di = 0
    cnt = 0
    for ex in range(nexp):
        for qt in range(4):
            gv, tg = gub[gi % 2], T_gub[gi % 2]
            gi += 1
            fs = slice(qt * 256, (qt + 1) * 256)
            P.dma("pool", gv[:, 0, :, :], wg_e[ex, :, fs].rearrange("(a p) f -> p a f", p=128), writes=[tg], owner=tg)
            P.dma("pool", gv[:, 1, :, :], wu_e[ex, :, fs].rearrange("(a p) f -> p a f", p=128), writes=[tg], owner=tg)
            for fc in range(2):
                for half in range(2):
                    hs = slice(half * 512, (half + 1) * 512)
                    pg, pu = (0, 1) if (cnt % 2 == 0) else (2, 3)
                    st_, tst = sgt[cnt % 2], T_sgt[cnt % 2]
                    cnt += 1
                    for k2, pi in ((0, pg), (1, pu)):
                        for dc in range(NDC):
                            P.op("pe", (lambda e, gv=gv, k2=k2, dc=dc, fc=fc, hs=hs, pi=pi: e.matmul(
                                psb[pi][:, :], lhsT=gv[:, k2, dc, fc * 128:(fc + 1) * 128], rhs=h2T[:, dc, hs],
                                start=(dc == 0), stop=(dc == NDC - 1))), reads=[tg, T_h2T], writes=[Tps[pi]])
                    P.op("act", (lambda e, pg=pg, st_=st_: e.activation(out=st_, in_=psb[pg][:, :], func=AF.Silu)), reads=[Tps[pg]], writes=[tst])
                    P.op("dve", (lambda e, pu=pu, st_=st_, qt=qt, fc=fc, hs=hs: e.tensor_tensor(out=actT[:, qt * 2 + fc, hs], in0=psb[pu][:, :], in1=st_, op=ALU.mult)),
                         reads=[Tps[pu], tst], writes=[T_actT])
        for cb in range(4):
            dv, td = dwb[di % 2], T_dwb[di % 2]
            di += 1
            P.dma("pool", dv, wd_e[ex, :, cb * 512:(cb + 1) * 512].rearrange("(a p) f -> p a f", p=128), writes=[td], owner=td)
            for n in range(8):
                pi = 4 + (n % 4)
                for fc in range(8):
                    P.op("pe", (lambda e, dv=dv, fc=fc, n=n, pi=pi: e.matmul(psb[pi][:, :], lhsT=actT[:, fc, n * 128:(n + 1) * 128], rhs=dv[:, fc, :],
                                                                            start=(fc == 0), stop=(fc == 7))), reads=[T_actT, td], writes=[Tps[pi]])
                P.op("dve", (lambda e, pi=pi, n=n, cb=cb, ex=ex: e.scalar_tensor_tensor(
                    out=yacc[:, n, cb * 512:(cb + 1) * 512], in0=psb[pi][:, :], scalar=Wt[:, n, ex:ex + 1], in1=yacc[:, n, cb * 512:(cb + 1) * 512],
                    op0=ALU.mult, op1=ALU.add)), reads=[Tps[pi], T_Wt], writes=[T_yacc[n]])
    P.barrier()

    o[0] = C_END
    fng = af(2048)
    x1r = [af(2048) for _ in range(2)]
    osb = [af(2048) for _ in range(2)]
    junk3 = ab(2048)
    st3 = af(16)
    T_fng, T_junk3, T_st3 = T("fng"), T("junk3"), T("st3")
    T_x1r = [T("x1r0"), T("x1r1")]
    T_osb = [T("osb0"), T("osb1")]
    assert o[0] <= 32520
    P.dma("sp", fng, rowsbig[:, 2048:4096], writes=[T_fng], owner=T_fng)
    for n in range(8):
        xr, txr = x1r[n % 2], T_x1r[n % 2]
        ob, tob = osb[n % 2], T_osb[n % 2]
        P.dma("sp", xr, x1s[n * 128:(n + 1) * 128, :], writes=[txr], owner=txr, deps=x1_stores)
        P.op("dve", (lambda e, n=n: e.tensor_tensor(out=yacc[:, n, :], in0=yacc[:, n, :], in1=g2b, op=ALU.mult)), reads=[T_gb], writes=[T_yacc[n]])
        P.op("dve", (lambda e, n=n, xr=xr: e.tensor_tensor(out=xr, in0=xr, in1=yacc[:, n, :], op=ALU.add)), reads=[T_yacc[n]], writes=[txr])
        P.op("act", (lambda e, xr=xr: e.activation(out=junk3, in_=xr, func=AF.Square, accum_out=st3[:, 0:1])), reads=[txr], writes=[T_junk3, T_st3])
        P.op("act", lambda e: e.activation(out=st3[:, 1:2], in_=st3[:, 0:1], func=AF.Sqrt, bias=small[:, 0:1], scale=1.0 / D), reads=[T_st3, T_small], writes=[T_st3])
        P.op("dve", lambda e: e.reciprocal(out=st3[:, 2:3], in_=st3[:, 1:2]), reads=[T_st3], writes=[T_st3])
        P.op("dve", (lambda e, xr=xr, ob=ob: e.scalar_tensor_tensor(out=ob, in0=xr, scalar=st3[:, 2:3], in1=fng, op0=ALU.mult, op1=ALU.mult)),
             reads=[txr, T_st3, T_fng], writes=[tob])
        final_waits.append(P.dma("sp", out[n * 128:(n + 1) * 128, :], ob, reads=[tob], owner=tob, store=True))
    with nc.allow_low_precision("bf16 matmuls"):
        P.finalize(final_waits)
    P.close()
    es.close()
    return nc


def make_inputs(core, inp, full=True):
    b, half = core // 2, core % 2
    flip = (half == 1)
    f32 = np.float32
    x = inp["x"][b]
    ctx = inp["ctx"][b]
    if flip:
        x = x[::-1]
        ctx = ctx[::-1]
    m = {}
    m["xl"] = np.ascontiguousarray(x, dtype=f32)
    m["ctxl"] = np.ascontiguousarray(ctx, dtype=f32)
    ccv = np.stack([inp["c"][b].reshape(16, 128).T, inp["c_ctx"].reshape(16, 128).T], axis=2)
    m["cc"] = np.ascontiguousarray(ccv.reshape(128, 32), dtype=f32)
    sel = [2, 0, 3, 1] if flip else [0, 2, 1, 3]
    rows = np.zeros((128, 128), f32)
    if full:
        gb = inp["mlstm_gate_b"][0]
        rows[:, 0:32] = np.concatenate([gb[g] for g in sel])[None, :]
        rows[:, 32:48] = inp["attn_sink"][0][None, :]
        rows[:, 48:56] = inp["b_router_grp"][0][None, :]
        rows[:, 56:120] = inp["b_router_exp"][0][None, :]
    m["rows"] = rows
    if not full:
        return m
    w_in = inp["w_in"][0]
    gcols = np.concatenate([OFF_GM + g * 8 + np.arange(8) for g in sel])
    m["wgate"] = pmaj(w_in[:, gcols])
    loc = np.arange(1152)
    pos = (SEQ - 1 - loc) if flip else loc
    pos = np.clip(pos, 0, SEQ - 1)
    row = (pos // 64).astype(f32)
    colp = (pos % 64).astype(f32)
    inv = (10000.0 ** (-np.arange(32, dtype=f32) / 32)).astype(f32)
    C = np.zeros((128, 1152), f32)
    S = np.zeros((128, 1152), f32)
    for p in range(128):
        ax = p // 64
        fi = p % 32
        ang = ((row if ax == 0 else colp) * inv[fi]).astype(f32)
        C[p] = np.cos(ang)
        sgn = -1.0 if (p % 64) < 32 else 1.0
        S[p] = sgn * np.sin(ang)
    m["ropec"] = C
    m["ropes"] = S
    return m


def shared_inputs(inp, full=True):
    f32 = np.float32
    s = {}
    w_mod = inp["w_mod"][0]
    s["wmod"] = np.stack([pmaj(w_mod[:, blk * 512:(blk + 1) * 512]) for blk in range(24)])
    bm = inp["b_mod"][0].reshape(96, 128).T
    vecs = np.zeros((128, 224), f32)
    vecs[:, 0:192] = np.repeat(bm, 2, axis=1)
    vecs[:, 192:208] = inp["norm1_g"][0].reshape(16, 128).T
    vecs[:, 208:224] = inp["norm2_g"][0].reshape(16, 128).T
    s["vecs"] = vecs
    cst = np.zeros((128, 1280), f32)
    ii = np.arange(128)
    cst[:, 0:128] = np.eye(128, dtype=f32)
    cst[:, 128:256] = (ii[:, None] <= ii[None, :])
    cst[:, 256:384] = (ii[:, None] >= ii[None, :])
    cst[:, 384:512] = 1.0
    prev = np.where(ii[:, None] <= ii[None, :], 0.0, NEG)
    nxt = np.where(ii[None, :] <= ii[:, None], 0.0, NEG)
    cst[:, 512:640] = prev
    cst[:, 768:896] = nxt
    cst[:, 896:1024] = NEG
    cst[:, 1152:1280] = nxt
    s["consts"] = cst
    if not full:
        return s
    w_in = inp["w_in"][0]
    rb = np.zeros((128, 4096), f32)
    rb[:, 0:2048] = inp["mlstm_norm_g"][0][None, :]
    rb[:, 2048:4096] = inp["final_norm_g"][None, :]
    s["rowsbig"] = rb
    wmh0, wmh1 = [], []
    for h in range(8):
        q = w_in[:, OFF_QM + h * 128: OFF_QM + (h + 1) * 128]
        k = w_in[:, OFF_KM + h * 128: OFF_KM + (h + 1) * 128]
        v = w_in[:, OFF_VM + h * 256: OFF_VM + (h + 1) * 256]
        og = w_in[:, OFF_OM + h * 256: OFF_OM + (h + 1) * 256]
        wmh0.append(pmaj(np.concatenate([q, k], 1)))
        wmh1.append(pmaj(np.concatenate([v, og], 1)))
    s["wmh0"] = np.stack(wmh0)
    s["wmh1"] = np.stack(wmh1)
    perm4 = rope_perm(4)
    perm1 = rope_perm(1)
    waq, waqp, wak = [], [], []
    for g in range(4):
        q = w_in[:, OFF_QA + g * 512: OFF_QA + (g + 1) * 512]
        k = w_in[:, OFF_KA + g * 128: OFF_KA + (g + 1) * 128]
        v = w_in[:, OFF_VA + g * 128: OFF_VA + (g + 1) * 128]
        waq.append(pmaj(q))
        waqp.append(pmaj(q[:, perm4]))
        wak.append(pmaj(np.concatenate([k, k[:, perm1], v, np.zeros((D, 128), f32)], 1)))
    s["waq"] = np.stack(waq)
    s["waqp"] = np.stack(waqp)
    s["wak"] = np.stack(wak)
    wz = []
    for j in range(16):
        cs = slice(j * 128, (j + 1) * 128)
        parts = [inp["w_br_m"][0][:, cs], inp["w_br_a"][0][:, cs],
                 w_in[:, OFF_GBR + j * 128: OFF_GBR + (j + 1) * 128],
                 w_in[:, OFF_GBR + 2048 + j * 128: OFF_GBR + 2048 + (j + 1) * 128]]
        wz.append(np.concatenate([pmaj(p_) for p_ in parts], 1))
    s["wz"] = np.stack(wz)
    s["wout"] = np.stack([pmaj(inp["w_out"][0][:, cb * 512:(cb + 1) * 512]) for cb in range(4)])
    s["wr"] = pmaj(np.concatenate([inp["w_router_grp"][0], inp["w_router_exp"][0]], 1))
    s["wg_e"] = np.ascontiguousarray(inp["w_exp_gate"][0])
    s["wu_e"] = np.ascontiguousarray(inp["w_exp_up"][0])
    s["wd_e"] = np.ascontiguousarray(inp["w_exp_down"][0])
    return s


_NC_CACHE = {}


def kernel(**inputs):
    inp = {k: np.asarray(v) for k, v in inputs.items()}
    if "nc" not in _NC_CACHE:
        _NC_CACHE["nc"] = build()
    nc = _NC_CACHE["nc"]
    sh = shared_inputs(inp)
    maps = []
    for core in range(8):
        m = make_inputs(core, inp)
        m.update(sh)
        maps.append(m)
    res = run_bass_kernel_spmd(nc, maps, core_ids=list(range(8)))
    outp = np.zeros((4, SEQ, D), np.float32)
    for core in range(8):
        b, half = core // 2, core % 2
        o_ = np.asarray(res.results[core]["out"], dtype=np.float32)
        if half == 0:
            outp[b, 0:OWN] = o_
        else:
            outp[b, OWN:] = o_[::-1]
    return outp
```
